# Optimizing a Trainium2 kernel written in Bass

```python
import math
import jax, jax.numpy as jnp
from jax import lax
import numpy as np

D_MODEL = 1024
BATCH = 4
SEQ = 4096
DEPTH = 4

EXPAND = 2
D_MIX = EXPAND * D_MODEL
D_CONV = D_MIX // 2
D_RET = D_MIX - D_CONV
CONV_GROUPS = 8
CONV_K = 3
RET_HEADS = 8
RET_HEAD_DIM = D_RET // RET_HEADS
CHUNK = 128
ROPE_BASE = 10000.0
NORM_EPS = 1e-6
IN_COLS = 4 * D_CONV + 4 * D_RET

kernel_name = "hybrid_shortconv_retention_parallel_heads"


def rms_norm(x, g, eps=NORM_EPS):
    xf = x.astype(jnp.float32)
    y = xf * lax.rsqrt(jnp.mean(xf * xf, axis=-1, keepdims=True) + eps)
    return (y * g.astype(jnp.float32)).astype(x.dtype)


def rotary_tables(positions):
    half = RET_HEAD_DIM // 2
    inv_freq = 1.0 / (ROPE_BASE ** (jnp.arange(half, dtype=jnp.float32) / half))
    ang = positions.astype(jnp.float32)[..., None] * inv_freq
    return jnp.cos(ang)[:, :, None, :], jnp.sin(ang)[:, :, None, :]


def apply_rotary(x, cos, sin):
    xf = x.astype(jnp.float32)
    x1, x2 = jnp.split(xf, 2, axis=-1)
    return jnp.concatenate([x1 * cos - x2 * sin, x2 * cos + x1 * sin], axis=-1).astype(x.dtype)


def causal_depthwise_conv(u, w):
    c = u.shape[-1]
    return lax.conv_general_dilated(
        u, w.astype(u.dtype)[:, None, :], window_strides=(1,), padding=[(CONV_K - 1, 0)],
        dimension_numbers=("NWC", "WIO", "NWC"), feature_group_count=c)


def retention_chunkwise(q, k, v, log_gamma):
    b, s, h, dh = q.shape
    n = s // CHUNK
    q = q.reshape(b, n, CHUNK, h, dh)
    k = k.reshape(b, n, CHUNK, h, dh) * (dh ** -0.5)
    v = v.reshape(b, n, CHUNK, h, dh)
    idx = jnp.arange(CHUNK, dtype=jnp.float32)
    diff = idx[:, None] - idx[None, :]
    causal = diff >= 0
    decay = jnp.where(causal[None], jnp.exp(log_gamma[:, None, None] * jnp.where(causal, diff, 0.0)[None]), 0.0)
    scores = jnp.einsum("bnihd,bnjhd->bnhij", q, k).astype(jnp.float32) * decay
    intra = jnp.einsum("bnhij,bnjhe->bnihe", scores, v.astype(jnp.float32))
    k_w = jnp.exp((CHUNK - 1 - idx)[:, None] * log_gamma[None, :])
    kv = jnp.einsum("bnjhd,bnjhe->nbhde", k.astype(jnp.float32) * k_w[:, :, None],
                    v.astype(jnp.float32))
    chunk_decay = jnp.exp(CHUNK * log_gamma)[None, :, None, None]

    def step(state, kv_n):
        return chunk_decay * state + kv_n, state

    init = jnp.zeros((b, h, dh, dh), jnp.float32)
    _, prev = lax.scan(step, init, kv)
    q_w = jnp.exp((idx + 1.0)[:, None] * log_gamma[None, :])
    cross = jnp.einsum("bnihd,nbhde->bnihe", q.astype(jnp.float32) * q_w[:, :, None], prev)
    return (intra + cross).reshape(b, s, h, dh)


def setup_inputs(seed: int = 0) -> dict:
    key = jax.random.key(seed)
    kx, kp, kn1, kw1, kc, kw2, kn2 = jax.random.split(key, 7)
    x = jax.random.normal(kx, (BATCH, SEQ, D_MODEL), jnp.float32)
    offsets = jax.random.randint(kp, (BATCH, 1), 0, 1024, dtype=jnp.int32)
    positions = offsets + jnp.arange(SEQ, dtype=jnp.int32)[None, :]
    pre_norm = 1.0 + 0.05 * jax.random.normal(kn1, (DEPTH, D_MODEL), jnp.float32)
    w_in = jax.random.normal(kw1, (DEPTH, D_MODEL, IN_COLS), jnp.float32) * D_MODEL ** -0.5
    conv_w = jax.random.normal(kc, (DEPTH, CONV_K, D_CONV), jnp.float32) * CONV_K ** -0.5
    w_out = jax.random.normal(kw2, (DEPTH, D_MIX, D_MODEL), jnp.float32) * D_MIX ** -0.5
    post_norm = 1.0 + 0.05 * jax.random.normal(kn2, (DEPTH, D_MODEL), jnp.float32)
    return {"x": x, "positions": positions, "pre_norm": pre_norm, "w_in": w_in,
            "conv_w": conv_w, "w_out": w_out, "post_norm": post_norm}


def reference(x, positions, pre_norm, w_in, conv_w, w_out, post_norm):
    b, s, _ = x.shape
    cos, sin = rotary_tables(positions)
    log_gamma = jnp.log1p(-jnp.exp2(-5.0 - jnp.arange(RET_HEADS, dtype=jnp.float32)))
    cuts = [D_CONV, 2 * D_CONV, 3 * D_CONV, 4 * D_CONV,
            4 * D_CONV + D_RET, 4 * D_CONV + 2 * D_RET, 4 * D_CONV + 3 * D_RET]
    for layer in range(DEPTH):
        h = rms_norm(x, pre_norm[layer])
        proj = jnp.einsum("bsd,dp->bsp", h, w_in[layer])
        c_b, c_c, c_x, c_z, r_q, r_k, r_v, r_z = jnp.split(proj, cuts, axis=-1)
        y_conv = c_b * causal_depthwise_conv(c_c * c_x, conv_w[layer])
        y_conv = y_conv * jax.nn.silu(c_z)
        q = apply_rotary(r_q.reshape(b, s, RET_HEADS, RET_HEAD_DIM), cos, sin)
        k = apply_rotary(r_k.reshape(b, s, RET_HEADS, RET_HEAD_DIM), cos, sin)
        v = r_v.reshape(b, s, RET_HEADS, RET_HEAD_DIM)
        ret = retention_chunkwise(q, k, v, log_gamma)
        ret = ret * lax.rsqrt(jnp.mean(ret * ret, axis=-1, keepdims=True) + NORM_EPS)
        y_ret = ret.reshape(b, s, D_RET).astype(h.dtype) * jax.nn.silu(r_z)
        mix = jnp.concatenate([y_conv.astype(h.dtype), y_ret], axis=-1)
        out = jnp.einsum("bse,ed->bsd", mix, w_out[layer])
        x = x + rms_norm(out, post_norm[layer]).astype(x.dtype)
    return x
```

```python
import math
import numpy as np
import concourse.bass as bass
import concourse.mybir as mybir
from concourse.bass_utils import run_bass_kernel_spmd

F32 = mybir.dt.float32
BF16 = mybir.dt.bfloat16
I32 = mybir.dt.int32
ALU = mybir.AluOpType
AF = mybir.ActivationFunctionType

D = 1024
KC = 8
T = 1024
NCH = T // 128
SEQ = 4096
NT = SEQ // T
DEPTH = 4
EPS = 1e-6
N_CORES = 4

C_ID = 0
C_MASK = 128
C_O128 = 256
C_O1024 = 384
C_QW = 512
C_KW = 520
C_INVF = 528
C_HALFPI = 592
C_EPS = 593
C_ONE = 594
C_TOT = 596

TWO_PI = 2.0 * math.pi
CW1 = 6.28125
CW2 = TWO_PI - CW1
PI_SAFE = 3.1415925


class _Op:
    __slots__ = ("eng", "fn", "deps", "dma", "n_dma", "tick", "sem", "has_consumer", "idx", "name")

    def __init__(self, eng, fn, dma, n_dma, name):
        self.eng = eng
        self.fn = fn
        self.deps = []
        self.dma = dma
        self.n_dma = n_dma
        self.tick = None
        self.sem = None
        self.has_consumer = False
        self.idx = None
        self.name = name


class Sched:
    ENGS = ("pe", "act", "dve", "pool", "sp")

    def __init__(self, nc, strict=True):
        self.nc = nc
        self.ops = []
        self.streams = {e: [] for e in self.ENGS}
        self.last_writer = {}
        self.readers = {}
        self.lastlock = {}
        self.strict = strict

    def op(self, eng, fn, reads=(), writes=(), dma=None, n_dma=1, name=""):
        o = _Op(eng, fn, dma, n_dma, name)
        deps = {}
        for b in reads:
            w = self.last_writer.get(b)
            if w is not None:
                deps[w.idx] = w
        for b in writes:
            w = self.last_writer.get(b)
            if w is not None:
                deps[w.idx] = w
            for r in self.readers.get(b, ()):
                deps[r.idx] = r
        banks = set()
        for b in list(reads) + list(writes):
            if isinstance(b, tuple) and b and b[0] == "ps":
                banks.add(b[1])
        for bk in banks:
            ll = self.lastlock.setdefault(bk, {})
            for f, w in ll.items():
                if f != eng:
                    deps[w.idx] = w
        o.deps = [deps[k] for k in sorted(deps)]
        o.idx = len(self.ops)
        for bk in banks:
            self.lastlock[bk][eng] = o
        self.ops.append(o)
        self.streams[eng].append(o)
        for b in reads:
            self.readers.setdefault(b, []).append(o)
        for b in writes:
            self.last_writer[b] = o
            self.readers[b] = []
        return o

    def emit(self, final_wait_ops=()):
        nc = self.nc
        eng_sem = {e: nc.alloc_semaphore(name=f"s_{e}") for e in self.ENGS}
        chan_sem = {}
        chan_cnt = {}
        for o in self.ops:
            for d in o.deps:
                if d.dma is not None:
                    d.has_consumer = True
                elif d.eng != o.eng or (self.strict and o.eng != "pe"):
                    d.has_consumer = True
        for o in final_wait_ops:
            o.has_consumer = True
        eng_cnt = {e: 0 for e in self.ENGS}
        for o in self.ops:
            if o.dma is not None:
                if o.dma not in chan_sem:
                    chan_sem[o.dma] = nc.alloc_semaphore(name=f"d_{o.dma}")
                    chan_cnt[o.dma] = 0
                chan_cnt[o.dma] += 16 * o.n_dma
                o.sem = chan_sem[o.dma]
                o.tick = chan_cnt[o.dma]
            elif o.has_consumer:
                eng_cnt[o.eng] += 1
                o.sem = eng_sem[o.eng]
                o.tick = eng_cnt[o.eng]
        strict = self.strict

        def run_stream(ename, engine, extra_final=()):
            waited = {}
            for o in self.streams[ename]:
                for d in o.deps:
                    if d.sem is None:
                        continue
                    if d.dma is None and d.eng == ename and not (strict and ename != "pe"):
                        continue
                    key = id(d.sem)
                    if waited.get(key, 0) >= d.tick:
                        continue
                    engine.wait_ge(d.sem, d.tick)
                    waited[key] = d.tick
                res = o.fn(engine)
                if o.dma is not None:
                    insts = res if isinstance(res, (list, tuple)) else [res]
                    assert len(insts) == o.n_dma, (o.name, len(insts), o.n_dma)
                    for ins in insts:
                        ins.then_inc(o.sem, 16)
                elif o.sem is not None:
                    assert res is not None, o.name
                    res.then_inc(o.sem, 1)
            for d in extra_final:
                engine.wait_ge(d.sem, d.tick)

        with nc.Block() as block:
            @block.tensor
            def _(e):
                run_stream("pe", e)

            @block.scalar
            def _(e):
                run_stream("act", e)

            @block.vector
            def _(e):
                run_stream("dve", e)

            @block.gpsimd
            def _(e):
                run_stream("pool", e)

            @block.sync
            def _(e):
                run_stream("sp", e, extra_final=final_wait_ops)


def _g128(h):
    lg = math.log1p(-(2.0 ** (-5.0 - h)))
    return math.exp(128.0 * lg)


def build(L, NT=NT):
    nc = bass.Bass("TRN2", target_bir_lowering=False)
    x_in = nc.dram_tensor("x_in", [NT, NCH, 128, D], F32, kind="ExternalInput").ap()
    y_out = nc.dram_tensor("y_out", [NT, NCH, 128, D], F32, kind="ExternalOutput").ap()
    posi_d = nc.dram_tensor("posi", [128, NT * NCH], I32, kind="ExternalInput").ap()
    w_in_d = nc.dram_tensor("w_in", [L, 16, 128, KC * 512], F32, kind="ExternalInput").ap()
    w_out_d = nc.dram_tensor("w_out", [L, 128, 16 * D], F32, kind="ExternalInput").ap()
    gpre_d = nc.dram_tensor("gpre", [128, L * 8], F32, kind="ExternalInput").ap()
    gpost_d = nc.dram_tensor("gpost", [128, L * 8], F32, kind="ExternalInput").ap()
    cw_d = nc.dram_tensor("cw", [128, L * 24], F32, kind="ExternalInput").ap()
    cst_d = nc.dram_tensor("cst", [128, C_TOT], F32, kind="ExternalInput").ap()

    A = nc.alloc_sbuf_tensor
    xT = A("xT", [128, KC, T], F32)
    hT = A("hT", [128, KC, T], BF16)
    mixT = A("mixT", [128, 16, T], BF16)
    wout = A("wout", [128, 16, D], BF16)
    win = [A(f"win{i}", [128, KC, 512], BF16) for i in range(3)]
    Ast = A("Ast", [128, L * 8, 128], F32)
    Ubf = [A(f"Ubf{i}", [128, 128], BF16) for i in range(4)]
    utail = A("utail", [128, L * 8, 2], F32)
    o_sb = A("o_sb", [128, 8, 256], F32)
    sqn = A("sqn", [128, 8, 256], BF16)
    xs = o_sb[:, 0:4, :].rearrange("p c t -> p (c t)")
    XS = [("o_sb", i) for i in range(4)]
    rstd_n = A("rstd_n", [128, 256], F32)
    rstd_p = [A(f"rstd_p{i}", [128, 128], F32) for i in range(2)]
    cosF = A("cosF", [128, NCH, 128], F32)
    sinS = A("sinS", [128, NCH, 128], F32)
    ang = A("ang", [128, 64], F32)
    ki = A("ki", [128, 64], I32)
    kfl = A("kfl", [128, 64], F32)
    posi = A("posi_sb", [128, NT * NCH], I32)
    posf = A("posf", [128, NT * NCH], F32)
    cst = A("cst_sb", [128, C_TOT], F32)
    gpre = A("gpre_sb", [128, L * 8], F32)
    gpost = A("gpost_sb", [128, L * 8], F32)
    cw = A("cw_sb", [128, L * 24], F32)
    ident_bf = A("ident_bf", [128, 128], BF16)
    o128 = A("o128", [128, 128], BF16)
    o1024 = A("o1024", [128, 128], BF16)
    cx_sb = A("cx_sb", [128, 512], F32)
    ubuf = [A(f"ubuf{i}", [128, 514], F32) for i in range(2)]
    gate = [A(f"gate{i}", [128, 512], F32) for i in range(2)]
    ycv = A("ycv", [128, 512], F32)
    zs = gate
    t1 = [A(f"t1_{i}", [128, 128], F32) for i in range(2)]
    t2 = [A(f"t2_{i}", [128, 128], F32) for i in range(2)]
    t3 = [A(f"t3_{i}", [128, 128], F32) for i in range(2)]
    t4 = [A(f"t4_{i}", [128, 128], F32) for i in range(2)]
    qr = [A(f"qr{i}", [128, 128], BF16) for i in range(2)]
    kr = [A(f"kr{i}", [128, 128], BF16) for i in range(2)]
    v_sb = [A(f"v_sb{i}", [128, 128], BF16) for i in range(6)]
    RT_sb = [A(f"RT_sb{i}", [128, 128], F32) for i in range(3)]
    qT_sb = [A(f"qT_sb{i}", [128, 128], BF16) for i in range(3)]
    kT_sb = [A(f"kT_sb{i}", [128, 128], BF16) for i in range(3)]
    PT = [A(f"PT{i}", [128, 128], BF16) for i in range(2)]
    sqr = [A(f"sqr{i}", [128, 128], BF16) for i in range(2)]
    rl = [A(f"rl{i}", [128, 128], F32) for i in range(2)]
    rn = [A(f"rn{i}", [128, 128], F32) for i in range(2)]

    P = nc.alloc_psum_tensor
    bank = [P(f"bank{i}", [128, 512], F32) for i in range(8)]
    bank4_bf = bank[4][:, :].bitcast(BF16)

    def pk(b, q0=0, q1=4):
        return [("ps", b, q) for q in range(q0, q1)]

    S = Sched(nc)

    S.op("sp", lambda e: e.dma_start(out=cst[:], in_=cst_d), writes=["cst"], dma="c0")
    S.op("sp", lambda e: e.dma_start(out=gpre[:], in_=gpre_d), writes=["gpre"], dma="c1")
    S.op("sp", lambda e: e.dma_start(out=gpost[:], in_=gpost_d), writes=["gpost"], dma="c2")
    S.op("sp", lambda e: e.dma_start(out=cw[:], in_=cw_d), writes=["cw"], dma="c3")
    S.op("sp", lambda e: e.dma_start(out=posi[:], in_=posi_d), writes=["posi"], dma="c4")
    S.op("dve", lambda e: e.tensor_copy(out=ident_bf[:], in_=cst[:, C_ID:C_ID + 128]), reads=["cst"], writes=["ident_bf"])
    S.op("dve", lambda e: e.tensor_copy(out=o128[:], in_=cst[:, C_O128:C_O128 + 128]), reads=["cst"], writes=["o128"])
    S.op("dve", lambda e: e.tensor_copy(out=o1024[:], in_=cst[:, C_O1024:C_O1024 + 128]), reads=["cst"], writes=["o1024"])
    S.op("dve", lambda e: e.tensor_copy(out=posf[:], in_=posi[:]), reads=["posi"], writes=["posf"])
    S.op("pool", lambda e: e.memset(Ast[:], 0.0), writes=[("A", i) for i in range(L * 8)])
    S.op("pool", lambda e: e.memset(utail[:], 0.0), writes=[("utail", i) for i in range(L * 8)])
    identf = cst[:, C_ID:C_ID + 128]
    maskT = cst[:, C_MASK:C_MASK + 128]
    invf = cst[:, C_INVF:C_INVF + 64]

    out_dmas = []
    head_seq = [(ti_, l_, hd_) for ti_ in range(NT) for l_ in range(L) for hd_ in range(16)]
    n_loaded = [0]

    def ensure_loaded(upto):
        while n_loaded[0] <= min(upto, len(head_seq) - 1):
            n = n_loaded[0]
            _, l_, hd_ = head_seq[n]
            i = n % 3
            S.op("pool", lambda e, l_=l_, hd_=hd_, i=i: e.dma_start(
                out=win[i][:], in_=w_in_d[l_, hd_].rearrange("p (k c) -> p k c", k=KC)),
                writes=[("win", i)], dma=f"win{i}")
            n_loaded[0] += 1

    def start_head(ti_, l_, hd_):
        n = (ti_ * L + l_) * 16 + hd_
        ensure_loaded(n + 2)
        S.op("pool", lambda e, l_=l_, hd_=hd_: e.dma_start(out=wout[:, hd_, :], in_=w_out_d[l_][:, hd_ * D:(hd_ + 1) * D]),
             writes=[("wout", hd_)], dma=f"wout{hd_}")
        return n % 3

    ensure_loaded(1)

    for ti in range(NT):
        xt_ps = [bank[i][:, :].rearrange("p (k c) -> p k c", k=4) for i in range(4)]
        xs2 = [o_sb[:, 0:4, :].rearrange("p c t -> p (c t)"), o_sb[:, 4:8, :].rearrange("p c t -> p (c t)")]
        XS2 = [[("o_sb", i) for i in range(4)], [("o_sb", 4 + i) for i in range(4)]]
        for c in range(NCH):
            p = c % 2
            S.op("sp", lambda e, ti=ti, c=c, p=p: e.dma_start(out=xs2[p], in_=x_in[ti, c]), writes=XS2[p], dma=f"xs{p}")
            for kc in range(KC):
                S.op("pe", lambda e, kc=kc, p=p: e.transpose(out=xt_ps[2 * p + kc // 4][:, kc % 4, :], in_=xs2[p][:, kc * 128:(kc + 1) * 128],
                                                             identity=identf),
                     reads=XS2[p] + ["cst"], writes=pk(2 * p + kc // 4, kc % 4, kc % 4 + 1))
            S.op("dve", lambda e, c=c, p=p: e.tensor_copy(out=xT[:, 0:4, c * 128:(c + 1) * 128], in_=xt_ps[2 * p]),
                 reads=pk(2 * p), writes=[("xT", c // 2)])
            S.op("act", lambda e, c=c, p=p: e.copy(out=xT[:, 4:8, c * 128:(c + 1) * 128], in_=xt_ps[2 * p + 1]),
                 reads=pk(2 * p + 1), writes=[("xT", c // 2)])
        for c in range(NCH):
            gc = ti * NCH + c
            S.op("dve", lambda e, gc=gc: e.tensor_scalar(out=ang[:], in0=invf, scalar1=posf[:, gc:gc + 1], scalar2=None, op0=ALU.mult),
                 reads=["cst", "posf"], writes=["ang"])
            S.op("dve", lambda e: e.tensor_scalar(out=ki[:], in0=ang[:], scalar1=1.0 / TWO_PI, scalar2=None, op0=ALU.mult),
                 reads=["ang"], writes=["ki"])
            S.op("dve", lambda e: e.tensor_copy(out=kfl[:], in_=ki[:]), reads=["ki"], writes=["kfl"])
            S.op("dve", lambda e: e.scalar_tensor_tensor(out=ang[:], in0=kfl[:], scalar=-CW1, in1=ang[:], op0=ALU.mult, op1=ALU.add),
                 reads=["kfl", "ang"], writes=["ang"])
            S.op("dve", lambda e: e.scalar_tensor_tensor(out=ang[:], in0=kfl[:], scalar=-CW2, in1=ang[:], op0=ALU.mult, op1=ALU.add),
                 reads=["kfl", "ang"], writes=["ang"])
            S.op("dve", lambda e, c=c: e.tensor_scalar(out=sinS[:, c, 64:128], in0=ang[:], scalar1=-PI_SAFE, scalar2=PI_SAFE,
                                                      op0=ALU.max, op1=ALU.min),
                 reads=["ang"], writes=["sinS"])
        S.op("act", lambda e: e.activation(out=cosF[:, :, 64:128], in_=sinS[:, :, 64:128], func=AF.Abs),
             reads=["sinS"], writes=["cosF"])
        S.op("act", lambda e: e.activation(out=sinS[:, :, 0:64], in_=sinS[:, :, 64:128], func=AF.Sin, scale=-1.0),
             reads=["sinS"], writes=["sinS"])
        S.op("act", lambda e: e.activation(out=sinS[:, :, 64:128], in_=sinS[:, :, 64:128], func=AF.Sin),
             reads=["sinS"], writes=["sinS"])
        S.op("act", lambda e: e.activation(out=cosF[:, :, 0:64], in_=cosF[:, :, 64:128], func=AF.Sin, scale=-1.0, bias=cst[:, C_HALFPI:C_HALFPI + 1]),
             reads=["cosF", "cst"], writes=["cosF"])
        S.op("act", lambda e: e.copy(out=cosF[:, :, 64:128], in_=cosF[:, :, 0:64]), reads=["cosF"], writes=["cosF"])

        for l in range(L):
            def prenorm_granule(l_, gi):
                q2 = gi % 2
                t0_ = gi * 128
                sbk = gi // 2
                stg = (cx_sb if q2 == 0 else ycv)[:, :].bitcast(BF16).rearrange("p (k t) -> p k t", k=8)
                skey = "cx_sb" if q2 == 0 else "ycv"
                ps = bank[2 + q2][:, 256:384]
                pkeys = pk(2 + q2, 2, 3)
                S.op("act", lambda e: e.activation(out=stg, in_=xT[:, :, t0_:t0_ + 128], func=AF.Square),
                     reads=[("xT", sbk)], writes=[skey])
                for kc in range(KC):
                    S.op("pe", lambda e, kc=kc: e.matmul(ps, lhsT=o1024[:], rhs=stg[:, kc, :], start=(kc == 0), stop=(kc == KC - 1),
                                                         skip_group_check=True),
                         reads=[skey, "o1024"], writes=pkeys)
                S.op("act", lambda e: e.activation(out=rstd_p[q2][:], in_=ps, func=AF.Ln, bias=cst[:, C_EPS:C_EPS + 1]),
                     reads=pkeys + ["cst"], writes=[("rstd_p", q2)])
                S.op("act", lambda e: e.activation(out=rstd_p[q2][:], in_=rstd_p[q2][:], func=AF.Exp, scale=-0.5),
                     reads=[("rstd_p", q2)], writes=[("rstd_p", q2)])
                for kc in range(KC):
                    S.op("dve", lambda e, kc=kc: e.scalar_tensor_tensor(
                        out=hT[:, kc, t0_:t0_ + 128], in0=xT[:, kc, t0_:t0_ + 128], scalar=gpre[:, l_ * 8 + kc:l_ * 8 + kc + 1],
                        in1=rstd_p[q2][:], op0=ALU.mult, op1=ALU.mult),
                         reads=[("xT", sbk), "gpre", ("rstd_p", q2)], writes=[("hT", sbk)])

            for sb in (range(4) if l == 0 else ()):
                t0 = sb * 256
                nh = sb % 2
                S.op("act", lambda e, t0=t0: e.activation(out=sqn[:], in_=xT[:, :, t0:t0 + 256], func=AF.Square),
                     reads=[("xT", sb)], writes=["sqn"])
                for kc in range(KC):
                    S.op("pe", lambda e, kc=kc, nh=nh: e.matmul(bank[nh][:, 0:256], lhsT=o1024[:], rhs=sqn[:, kc, :],
                                                               start=(kc == 0), stop=(kc == KC - 1)),
                         reads=["sqn", "o1024"], writes=pk(nh, 0, 2))
                S.op("act", lambda e, nh=nh: e.activation(out=rstd_n[:], in_=bank[nh][:, 0:256], func=AF.Ln,
                                                         bias=cst[:, C_EPS:C_EPS + 1]),
                     reads=pk(nh, 0, 2) + ["cst"], writes=["rstd_n"])
                S.op("act", lambda e: e.activation(out=rstd_n[:], in_=rstd_n[:], func=AF.Exp, scale=-0.5),
                     reads=["rstd_n"], writes=["rstd_n"])
                for kc in range(KC):
                    S.op("dve", lambda e, kc=kc, t0=t0, l=l: e.scalar_tensor_tensor(
                        out=hT[:, kc, t0:t0 + 256], in0=xT[:, kc, t0:t0 + 256], scalar=gpre[:, l * 8 + kc:l * 8 + kc + 1],
                        in1=rstd_n[:], op0=ALU.mult, op1=ALU.mult),
                         reads=[("xT", sb), "gpre", "rstd_n"], writes=[("hT", sb)])

            conv_units = [(g, b) for g in range(8) for b in range(2)]
            wbuf_of = {}

            def conv_c0(u):
                g, b = conv_units[u]
                if b == 0:
                    wbuf_of[g] = start_head(ti, l, g)
                wi = wbuf_of[g]
                B = [0, 1, 2, 3] if u % 2 == 0 else [4, 5, 6, 7]
                for s in range(4):
                    for kc in range(KC):
                        S.op("pe", lambda e, s=s, kc=kc, wi=wi, b=b, B=B: e.matmul(
                            bank[B[s]][:, :], lhsT=win[wi][:, kc, s * 128:(s + 1) * 128], rhs=hT[:, kc, b * 512:(b + 1) * 512],
                            start=(kc == 0), stop=(kc == KC - 1)),
                             reads=[("win", wi), ("hT", 2 * b), ("hT", 2 * b + 1)], writes=pk(B[s]))

            def conv_c1(u):
                g, b = conv_units[u]
                B = [0, 1, 2, 3] if u % 2 == 0 else [4, 5, 6, 7]
                p = u % 2
                li = l * 8 + g
                S.op("act", lambda e, B=B: e.copy(out=cx_sb[:], in_=bank[B[2]][:, :]), reads=pk(B[2]), writes=["cx_sb"])
                S.op("act", lambda e, B=B, p=p: e.activation(out=gate[p][:], in_=bank[B[3]][:, :], func=AF.Exp, scale=-1.0),
                     reads=pk(B[3]), writes=[("gate", p)])
                S.op("act", lambda e, p=p: e.activation(out=gate[p][:], in_=gate[p][:], func=AF.Ln, bias=cst[:, C_ONE:C_ONE + 1]),
                     reads=[("gate", p), "cst"], writes=[("gate", p)])
                S.op("act", lambda e, p=p: e.activation(out=gate[p][:], in_=gate[p][:], func=AF.Exp, scale=-1.0),
                     reads=[("gate", p)], writes=[("gate", p)])
                if b == 0:
                    S.op("pool", lambda e, p=p, li=li: e.tensor_copy(out=ubuf[p][:, 0:2], in_=utail[:, li, :]),
                         reads=[("utail", li)], writes=[("ubuf", p)])
                else:
                    S.op("pool", lambda e, p=p: e.tensor_copy(out=ubuf[p][:, 0:2], in_=ubuf[1 - p][:, 512:514]),
                         reads=[("ubuf", 1 - p)], writes=[("ubuf", p)])
                S.op("dve", lambda e, B=B, p=p: e.tensor_tensor(out=ubuf[p][:, 2:514], in0=bank[B[1]][:, :], in1=cx_sb[:], op=ALU.mult),
                     reads=pk(B[1]) + ["cx_sb"], writes=[("ubuf", p)])
                if b == 1:
                    S.op("pool", lambda e, p=p, li=li: e.tensor_copy(out=utail[:, li, :], in_=ubuf[p][:, 512:514]),
                         reads=[("ubuf", p)], writes=[("utail", li)])
                S.op("dve", lambda e, B=B, p=p: e.tensor_tensor(out=gate[p][:], in0=bank[B[3]][:, :], in1=gate[p][:], op=ALU.mult),
                     reads=pk(B[3]) + [("gate", p)], writes=[("gate", p)])
                S.op("dve", lambda e, B=B, p=p: e.tensor_tensor(out=gate[p][:], in0=bank[B[0]][:, :], in1=gate[p][:], op=ALU.mult),
                     reads=pk(B[0]) + [("gate", p)], writes=[("gate", p)])

            def conv_c2(u):
                g, b = conv_units[u]
                p = u % 2
                li = l * 8 + g
                base = li * 3
                S.op("act", lambda e, p=p, base=base: e.activation(out=ycv[:], in_=ubuf[p][:, 2:514], func=AF.Copy,
                                                                  scale=cw[:, base + 2:base + 3]),
                     reads=[("ubuf", p), "cw"], writes=["ycv"])
                S.op("dve", lambda e, p=p, base=base: e.scalar_tensor_tensor(out=ycv[:], in0=ubuf[p][:, 1:513], scalar=cw[:, base + 1:base + 2],
                                                                             in1=ycv[:], op0=ALU.mult, op1=ALU.add),
                     reads=[("ubuf", p), "cw", "ycv"], writes=["ycv"])
                S.op("dve", lambda e, p=p, base=base: e.scalar_tensor_tensor(out=ycv[:], in0=ubuf[p][:, 0:512], scalar=cw[:, base:base + 1],
                                                                             in1=ycv[:], op0=ALU.mult, op1=ALU.add),
                     reads=[("ubuf", p), "cw", "ycv"], writes=["ycv"])
                S.op("pool", lambda e, p=p, g=g, b=b: e.tensor_tensor(out=mixT[:, g, b * 512:(b + 1) * 512], in0=ycv[:], in1=gate[p][:], op=ALU.mult),
                     reads=["ycv", ("gate", p)], writes=[("mixT", g, 2 * b), ("mixT", g, 2 * b + 1)])

            stages = [conv_c0, conv_c1, conv_c2]
            nU = len(conv_units)
            for step in range(nU + len(stages) - 1):
                for k in range(len(stages) - 1, -1, -1):
                    u = step - k
                    if 0 <= u < nU:
                        stages[k](u)

            ret_units = [(h, c) for h in range(8) for c in range(NCH)]

            def u_init(slot, h):
                li_ = l * 8 + h
                S.op("dve", lambda e: e.tensor_scalar(out=Ubf[slot][:], in0=Ast[:, li_, :], scalar1=_g128(h), scalar2=None, op0=ALU.mult),
                     reads=[("A", li_)], writes=[("U", slot)])

            def ret_b0(u):
                h, c = ret_units[u]
                if c == 0:
                    wbuf_of[8 + h] = start_head(ti, l, 8 + h)
                wi = wbuf_of[8 + h]
                tb = u % 2
                for kc in range(KC):
                    S.op("pe", lambda e, kc=kc: e.matmul(
                        bank[tb][:, 0:384], lhsT=hT[:, kc, c * 128:(c + 1) * 128], rhs=win[wi][:, kc, 0:384],
                        start=(kc == 0), stop=(kc == KC - 1)),
                         reads=[("win", wi), ("hT", c // 2)], writes=pk(tb, 0, 3))

            def ret_b1(u):
                h, c = ret_units[u]
                tb = u % 2
                r2 = u % 2
                r6 = u % 6
                TM = bank[tb]
                rd = pk(tb, 0, 3)
                qcol = cst[:, C_QW + h:C_QW + h + 1]
                kcol = cst[:, C_KW + h:C_KW + h + 1]
                S.op("act", lambda e: e.copy(out=v_sb[r6][:], in_=TM[:, 256:384]), reads=rd, writes=[("v_sb", r6)])
                S.op("dve", lambda e: e.scalar_tensor_tensor(out=t1[r2][:], in0=TM[:, 0:128], scalar=qcol, in1=cosF[:, c, :],
                                                             op0=ALU.mult, op1=ALU.mult),
                     reads=rd + ["cst", "cosF"], writes=[("t1", r2)])
                S.op("dve", lambda e: e.scalar_tensor_tensor(
                    out=t2[r2][:, :].rearrange("p (a b) -> p a b", a=2), in0=TM[:, 0:128].rearrange("p (a b) -> p a b", a=2)[:, ::-1, :],
                    scalar=qcol, in1=sinS[:, c, :].rearrange("p (a b) -> p a b", a=2), op0=ALU.mult, op1=ALU.mult),
                     reads=rd + ["cst", "sinS"], writes=[("t2", r2)])
                S.op("dve", lambda e: e.scalar_tensor_tensor(out=t3[r2][:], in0=TM[:, 128:256], scalar=kcol, in1=cosF[:, c, :],
                                                             op0=ALU.mult, op1=ALU.mult),
                     reads=rd + ["cst", "cosF"], writes=[("t3", r2)])
                S.op("dve", lambda e: e.scalar_tensor_tensor(
                    out=t4[r2][:, :].rearrange("p (a b) -> p a b", a=2), in0=TM[:, 128:256].rearrange("p (a b) -> p a b", a=2)[:, ::-1, :],
                    scalar=kcol, in1=sinS[:, c, :].rearrange("p (a b) -> p a b", a=2), op0=ALU.mult, op1=ALU.mult),
                     reads=rd + ["cst", "sinS"], writes=[("t4", r2)])

            def ret_b2(u):
                h, c = ret_units[u]
                r2 = u % 2
                S.op("pool", lambda e: e.tensor_tensor(out=qr[r2][:], in0=t1[r2][:], in1=t2[r2][:], op=ALU.add),
                     reads=[("t1", r2), ("t2", r2)], writes=[("qr", r2)])
                S.op("pool", lambda e: e.tensor_tensor(out=kr[r2][:], in0=t3[r2][:], in1=t4[r2][:], op=ALU.add),
                     reads=[("t3", r2), ("t4", r2)], writes=[("kr", r2)])

            def ret_b3(u):
                h, c = ret_units[u]
                r2 = u % 2
                r3 = u % 3
                r6 = u % 6
                li = l * 8 + h
                g = _g128(h)
                qT_ps = bank4_bf[:, 0:128]
                kT_ps = bank4_bf[:, 128:256]
                KV = bank[5][:, 128:256]
                S.op("pe", lambda e: e.transpose(out=qT_ps, in_=qr[r2][:], identity=ident_bf[:]),
                     reads=[("qr", r2), "ident_bf"], writes=pk(4, 0, 1))
                S.op("pe", lambda e: e.transpose(out=kT_ps, in_=kr[r2][:], identity=ident_bf[:]),
                     reads=[("kr", r2), "ident_bf"], writes=pk(4, 1, 2))
                S.op("pe", lambda e: e.matmul(KV, lhsT=kr[r2][:], rhs=v_sb[r6][:], start=True, stop=True, skip_group_check=True),
                     reads=[("kr", r2), ("v_sb", r6)], writes=pk(5, 1, 2))
                S.op("act", lambda e: e.copy(out=qT_sb[r3][:], in_=qT_ps), reads=pk(4, 0, 1), writes=[("qT_sb", r3)])
                S.op("act", lambda e: e.copy(out=kT_sb[r2][:], in_=kT_ps), reads=pk(4, 1, 2), writes=[("kT_sb", r2)])
                S.op("dve", lambda e: e.scalar_tensor_tensor(out=Ast[:, li, :], in0=Ast[:, li, :], scalar=g, in1=KV, op0=ALU.mult, op1=ALU.add),
                     reads=[("A", li)] + pk(5, 1, 2), writes=[("A", li)])
                if c < NCH - 1:
                    S.op("dve", lambda e: e.tensor_scalar(out=Ubf[(u + 1) % 4][:], in0=Ast[:, li, :], scalar1=g, scalar2=None, op0=ALU.mult),
                         reads=[("A", li)], writes=[("U", (u + 1) % 4)])
                elif h < 7:
                    u_init((u + 1) % 4, h + 1)
                zb = (u // 4) % 2
                if c % 4 == 0:
                    b = c // 4
                    wi = wbuf_of[8 + h]
                    for kc in range(KC):
                        S.op("pe", lambda e, kc=kc: e.matmul(bank[3][:, :], lhsT=win[wi][:, kc, 384:512], rhs=hT[:, kc, b * 512:(b + 1) * 512],
                                                             start=(kc == 0), stop=(kc == KC - 1)),
                             reads=[("win", wi), ("hT", 2 * b), ("hT", 2 * b + 1)], writes=pk(3))
                    S.op("act", lambda e: e.activation(out=zs[zb][:], in_=bank[3][:, :], func=AF.Exp, scale=-1.0),
                         reads=pk(3), writes=[("gate", zb)])
                elif c % 4 == 1:
                    S.op("act", lambda e: e.activation(out=zs[zb][:], in_=zs[zb][:], func=AF.Ln, bias=cst[:, C_ONE:C_ONE + 1]),
                         reads=[("gate", zb), "cst"], writes=[("gate", zb)])
                elif c % 4 == 2:
                    S.op("act", lambda e: e.activation(out=zs[zb][:], in_=zs[zb][:], func=AF.Exp, scale=-1.0),
                         reads=[("gate", zb)], writes=[("gate", zb)])
                else:
                    S.op("dve", lambda e: e.tensor_tensor(out=zs[zb][:], in0=bank[3][:, :], in1=zs[zb][:], op=ALU.mult),
                         reads=pk(3) + [("gate", zb)], writes=[("gate", zb)])

            def ret_b4(u):
                h, c = ret_units[u]
                r2 = u % 2
                r3 = u % 3
                ST = bank[5][:, 0:128]
                S.op("pe", lambda e: e.matmul(ST, lhsT=kT_sb[r2][:], rhs=qT_sb[r3][:], start=True, stop=True, skip_group_check=True),
                     reads=[("kT_sb", r2), ("qT_sb", r3)], writes=pk(5, 0, 1))
                S.op("dve", lambda e: e.tensor_tensor(out=PT[r2][:], in0=ST, in1=maskT, op=ALU.mult),
                     reads=pk(5, 0, 1) + ["cst"], writes=[("PT", r2)])

            def ret_b5(u):
                h, c = ret_units[u]
                r2 = u % 2
                r3 = u % 3
                r6 = u % 6
                RT = bank[6 + r2][:, 0:128]
                kRT = pk(6 + r2, 0, 1)
                S.op("pe", lambda e: e.matmul(RT, lhsT=v_sb[r6][:], rhs=PT[r2][:], start=True, stop=False),
                     reads=[("v_sb", r6), ("PT", r2)], writes=kRT)
                S.op("pe", lambda e: e.matmul(RT, lhsT=Ubf[u % 4][:], rhs=qT_sb[r3][:], start=False, stop=True),
                     reads=[("U", u % 4), ("qT_sb", r3)], writes=kRT)

            def ret_b6(u):
                r2 = u % 2
                r3 = u % 3
                RT = bank[6 + r2][:, 0:128]
                kRT = pk(6 + r2, 0, 1)
                S.op("act", lambda e: e.activation(out=sqr[r2][:], in_=RT, func=AF.Square), reads=kRT, writes=[("sqr", r2)])
                S.op("act", lambda e: e.copy(out=RT_sb[r3][:], in_=RT), reads=kRT, writes=[("RT_sb", r3)])

            def ret_b7(u):
                r2 = u % 2
                RS = bank[2][:, 0:128]
                S.op("pe", lambda e: e.matmul(RS, lhsT=o128[:], rhs=sqr[r2][:], start=True, stop=True),
                     reads=["o128", ("sqr", r2)], writes=pk(2, 0, 1))
                S.op("act", lambda e: e.activation(out=rl[r2][:], in_=RS, func=AF.Ln, bias=cst[:, C_EPS:C_EPS + 1]),
                     reads=pk(2, 0, 1) + ["cst"], writes=[("rl", r2)])
                S.op("act", lambda e: e.activation(out=rl[r2][:], in_=rl[r2][:], func=AF.Exp, scale=-0.5),
                     reads=[("rl", r2)], writes=[("rl", r2)])

            def ret_b8(u):
                h, c = ret_units[u]
                r2 = u % 2
                r3 = u % 3
                zb = (u // 4) % 2
                S.op("pool", lambda e: e.tensor_tensor(out=rn[r2][:], in0=RT_sb[r3][:], in1=rl[r2][:], op=ALU.mult),
                     reads=[("RT_sb", r3), ("rl", r2)], writes=[("rn", r2)])
                S.op("pool", lambda e: e.tensor_tensor(out=mixT[:, 8 + h, c * 128:(c + 1) * 128], in0=rn[r2][:],
                                                       in1=zs[zb][:, (c % 4) * 128:(c % 4 + 1) * 128], op=ALU.mult),
                     reads=[("rn", r2), ("gate", zb)], writes=[("mixT", 8 + h, c // 2)])

            u_init(0, 0)
            stages = [ret_b0, ret_b1, ret_b2, ret_b3, ret_b4, ret_b5, ret_b6, ret_b7, ret_b8]
            order = [2, 8, 3, 7, 6, 5, 4, 1, 0]
            nU = len(ret_units)
            for step in range(nU + len(stages) - 1):
                for k in order:
                    u = step - k
                    if 0 <= u < nU:
                        stages[k](u)

            for sb in range(4):
                t0 = sb * 256
                nh = sb % 2
                for cc in range(8):
                    ob = bank[cc][:, 0:256]
                    okeys = pk(cc, 0, 2)
                    for fc in range(16):
                        S.op("pe", lambda e, ob=ob, fc=fc, cc=cc, t0=t0: e.matmul(
                            ob, lhsT=wout[:, fc, cc * 128:(cc + 1) * 128], rhs=mixT[:, fc, t0:t0 + 256],
                            start=(fc == 0), stop=(fc == 15)),
                             reads=[("wout", fc), ("mixT", fc, sb)], writes=okeys)
                    S.op("act", lambda e, ob=ob, cc=cc: e.copy(out=o_sb[:, cc, :], in_=ob), reads=okeys, writes=[("o_sb", cc)])
                    S.op("act", lambda e, ob=ob, cc=cc: e.activation(out=sqn[:, cc, :], in_=ob, func=AF.Square), reads=okeys, writes=["sqn"])
                for cc in range(8):
                    S.op("pe", lambda e, cc=cc, nh=nh: e.matmul(bank[nh][:, 256:512], lhsT=o1024[:], rhs=sqn[:, cc, :],
                                                               start=(cc == 0), stop=(cc == 7)),
                         reads=["sqn", "o1024"], writes=pk(nh, 2, 4))
                S.op("act", lambda e, nh=nh: e.activation(out=rstd_n[:], in_=bank[nh][:, 256:512], func=AF.Ln,
                                                         bias=cst[:, C_EPS:C_EPS + 1]),
                     reads=pk(nh, 2, 4) + ["cst"], writes=["rstd_n"])
                S.op("act", lambda e: e.activation(out=rstd_n[:], in_=rstd_n[:], func=AF.Exp, scale=-0.5),
                     reads=["rstd_n"], writes=["rstd_n"])
                for cc in range(8):
                    S.op("dve", lambda e, cc=cc, l=l: e.scalar_tensor_tensor(
                        out=o_sb[:, cc, :], in0=o_sb[:, cc, :], scalar=gpost[:, l * 8 + cc:l * 8 + cc + 1], in1=rstd_n[:],
                        op0=ALU.mult, op1=ALU.mult),
                         reads=[("o_sb", cc), "gpost", "rstd_n"], writes=[("o_sb", cc)])
                    S.op("pool", lambda e, cc=cc, t0=t0: e.tensor_tensor(out=xT[:, cc, t0:t0 + 256], in0=xT[:, cc, t0:t0 + 256],
                                                                        in1=o_sb[:, cc, :], op=ALU.add),
                         reads=[("o_sb", cc), ("xT", sb)], writes=[("xT", sb)])
                if l + 1 < L:
                    prenorm_granule(l + 1, 2 * sb)
                    prenorm_granule(l + 1, 2 * sb + 1)

        for c in range(NCH):
            p = c % 2
            for kc in range(KC):
                S.op("pe", lambda e, kc=kc, c=c, p=p: e.transpose(out=xt_ps[2 * p + kc // 4][:, kc % 4, :], in_=xT[:, kc, c * 128:(c + 1) * 128],
                                                                  identity=identf),
                     reads=[("xT", c // 2), "cst"], writes=pk(2 * p + kc // 4, kc % 4, kc % 4 + 1))
            S.op("dve", lambda e, p=p: e.tensor_copy(out=xs2[p][:, 0:512], in_=bank[2 * p][:, :]), reads=pk(2 * p), writes=XS2[p])
            S.op("act", lambda e, p=p: e.copy(out=xs2[p][:, 512:1024], in_=bank[2 * p + 1][:, :]), reads=pk(2 * p + 1), writes=XS2[p])
            out_dmas.append(S.op("sp", lambda e, ti=ti, c=c, p=p: e.dma_start(out=y_out[ti, c], in_=xs2[p]), reads=XS2[p], dma=f"ys{p}"))

    S.emit(final_wait_ops=[out_dmas[-2], out_dmas[-1]])
    return nc


def _consts():
    cst = np.zeros((128, C_TOT), np.float32)
    cst[:, C_ID:C_ID + 128] = np.eye(128, dtype=np.float32)
    j = np.arange(128)[:, None]
    i = np.arange(128)[None, :]
    cst[:, C_MASK:C_MASK + 128] = (i >= j).astype(np.float32)
    cst[:, C_O128:C_O128 + 128] = 1.0 / 128.0
    cst[:, C_O1024:C_O1024 + 128] = 1.0 / 1024.0
    lg = np.log1p(-np.exp2(-5.0 - np.arange(8, dtype=np.float64)))
    idx = np.arange(128, dtype=np.float64)[:, None]
    cst[:, C_QW:C_QW + 8] = np.exp((idx + 1.0) * lg[None, :]).astype(np.float32)
    cst[:, C_KW:C_KW + 8] = (np.exp(-(idx + 1.0) * lg[None, :]) * (128.0 ** -0.5)).astype(np.float32)
    half = 64
    inv_freq = (1.0 / (np.float32(10000.0) ** (np.arange(half, dtype=np.float32) / np.float32(half)))).astype(np.float32)
    cst[:, C_INVF:C_INVF + 64] = inv_freq[None, :]
    cst[:, C_HALFPI] = math.pi / 2
    cst[:, C_EPS] = EPS
    cst[:, C_ONE] = 1.0
    return cst


_NC_CACHE = {}


def _get_nc(L):
    if L not in _NC_CACHE:
        _NC_CACHE[L] = build(L)
    return _NC_CACHE[L]


def _layout_weights(w_in, w_out, pre_norm, post_norm, conv_w):
    L = w_in.shape[0]
    wi = np.ascontiguousarray(
        w_in.reshape(L, 8, 128, 2, 4, 8, 128).transpose(0, 3, 5, 2, 1, 4, 6)).reshape(L, 16, 128, KC * 512)
    wo = np.ascontiguousarray(w_out.reshape(L, 16, 128, D).transpose(0, 2, 1, 3)).reshape(L, 128, 16 * D)
    gpre = np.ascontiguousarray(pre_norm.reshape(L, 8, 128).transpose(2, 0, 1)).reshape(128, L * 8)
    gpost = np.ascontiguousarray(post_norm.reshape(L, 8, 128).transpose(2, 0, 1)).reshape(128, L * 8)
    cw = np.ascontiguousarray(conv_w.reshape(L, 3, 8, 128).transpose(3, 0, 2, 1)).reshape(128, L * 24)
    return wi, wo, gpre, gpost, cw


FUSED = True


def kernel(x, positions, pre_norm, w_in, conv_w, w_out, post_norm):
    x = np.asarray(x, np.float32)
    positions = np.asarray(positions, np.int32)
    B = x.shape[0]
    cst = _consts()
    posl = [np.ascontiguousarray(positions[b].reshape(NT * NCH, 128).T) for b in range(B)]
    groups = [list(range(DEPTH))] if FUSED else [[l] for l in range(DEPTH)]
    cur = x
    for layers in groups:
        L = len(layers)
        nc = _get_nc(L)
        wi, wo, gpre, gpost, cw = _layout_weights(
            np.asarray(w_in, np.float32)[layers], np.asarray(w_out, np.float32)[layers],
            np.asarray(pre_norm, np.float32)[layers], np.asarray(post_norm, np.float32)[layers],
            np.asarray(conv_w, np.float32)[layers])
        in_maps = []
        for b in range(B):
            in_maps.append({"x_in": np.ascontiguousarray(cur[b].reshape(NT, NCH, 128, D)), "posi": posl[b], "w_in": wi,
                            "w_out": wo, "gpre": gpre, "gpost": gpost, "cw": cw, "cst": cst})
        res = run_bass_kernel_spmd(nc, in_maps, core_ids=list(range(B)))
        cur = np.stack([np.asarray(r["y_out"]).reshape(SEQ, D) for r in res.results], axis=0)
    return cur.astype(np.float32)
```

```python
import math
import numpy as np
import concourse.bass as bass
import concourse.mybir as mybir
from concourse.bass_utils import run_bass_kernel_spmd

F32 = mybir.dt.float32
BF16 = mybir.dt.bfloat16
I32 = mybir.dt.int32
ALU = mybir.AluOpType
AF = mybir.ActivationFunctionType

D = 1024
KC = 8
T = 1024
NCH = T // 128
SEQ = 4096
NT = SEQ // T
DEPTH = 4
EPS = 1e-6
N_CORES = 4

C_ID = 0
C_MASK = 128
C_O128 = 256
C_O1024 = 384
C_QW = 512
C_KW = 520
C_INVF = 528
C_HALFPI = 592
C_EPS = 593
C_ONE = 594
C_TOT = 596

TWO_PI = 2.0 * math.pi
CW1 = 6.28125
CW2 = TWO_PI - CW1
PI_SAFE = 3.1415925


class _Op:
    __slots__ = ("eng", "fn", "deps", "dma", "n_dma", "tick", "sem", "has_consumer", "idx", "name")

    def __init__(self, eng, fn, dma, n_dma, name):
        self.eng = eng
        self.fn = fn
        self.deps = []
        self.dma = dma
        self.n_dma = n_dma
        self.tick = None
        self.sem = None
        self.has_consumer = False
        self.idx = None
        self.name = name


class Sched:
    ENGS = ("pe", "act", "dve", "pool", "sp")

    def __init__(self, nc, strict=True):
        self.nc = nc
        self.ops = []
        self.streams = {e: [] for e in self.ENGS}
        self.last_writer = {}
        self.readers = {}
        self.lastlock = {}
        self.strict = strict

    def op(self, eng, fn, reads=(), writes=(), dma=None, n_dma=1, name=""):
        o = _Op(eng, fn, dma, n_dma, name)
        deps = {}
        for b in reads:
            w = self.last_writer.get(b)
            if w is not None:
                deps[w.idx] = w
        for b in writes:
            w = self.last_writer.get(b)
            if w is not None:
                deps[w.idx] = w
            for r in self.readers.get(b, ()):
                deps[r.idx] = r
        banks = set()
        for b in list(reads) + list(writes):
            if isinstance(b, tuple) and b and b[0] == "ps":
                banks.add(b[1])
        for bk in banks:
            ll = self.lastlock.setdefault(bk, {})
            for f, w in ll.items():
                if f != eng:
                    deps[w.idx] = w
        o.deps = [deps[k] for k in sorted(deps)]
        o.idx = len(self.ops)
        for bk in banks:
            self.lastlock[bk][eng] = o
        self.ops.append(o)
        self.streams[eng].append(o)
        for b in reads:
            self.readers.setdefault(b, []).append(o)
        for b in writes:
            self.last_writer[b] = o
            self.readers[b] = []
        return o

    def emit(self, final_wait_ops=()):
        nc = self.nc
        eng_sem = {e: nc.alloc_semaphore(name=f"s_{e}") for e in self.ENGS}
        chan_sem = {}
        chan_cnt = {}
        for o in self.ops:
            for d in o.deps:
                if d.dma is not None:
                    d.has_consumer = True
                elif d.eng != o.eng or (self.strict and o.eng != "pe"):
                    d.has_consumer = True
        for o in final_wait_ops:
            o.has_consumer = True
        eng_cnt = {e: 0 for e in self.ENGS}
        for o in self.ops:
            if o.dma is not None:
                if o.dma not in chan_sem:
                    chan_sem[o.dma] = nc.alloc_semaphore(name=f"d_{o.dma}")
                    chan_cnt[o.dma] = 0
                chan_cnt[o.dma] += 16 * o.n_dma
                o.sem = chan_sem[o.dma]
                o.tick = chan_cnt[o.dma]
            elif o.has_consumer:
                eng_cnt[o.eng] += 1
                o.sem = eng_sem[o.eng]
                o.tick = eng_cnt[o.eng]
        strict = self.strict

        def run_stream(ename, engine, extra_final=()):
            waited = {}
            for o in self.streams[ename]:
                for d in o.deps:
                    if d.sem is None:
                        continue
                    if d.dma is None and d.eng == ename and not (strict and ename != "pe"):
                        continue
                    key = id(d.sem)
                    if waited.get(key, 0) >= d.tick:
                        continue
                    engine.wait_ge(d.sem, d.tick)
                    waited[key] = d.tick
                res = o.fn(engine)
                if o.dma is not None:
                    insts = res if isinstance(res, (list, tuple)) else [res]
                    assert len(insts) == o.n_dma, (o.name, len(insts), o.n_dma)
                    for ins in insts:
                        ins.then_inc(o.sem, 16)
                elif o.sem is not None:
                    assert res is not None, o.name
                    res.then_inc(o.sem, 1)
            for d in extra_final:
                engine.wait_ge(d.sem, d.tick)

        with nc.Block() as block:
            @block.tensor
            def _(e):
                run_stream("pe", e)

            @block.scalar
            def _(e):
                run_stream("act", e)

            @block.vector
            def _(e):
                run_stream("dve", e)

            @block.gpsimd
            def _(e):
                run_stream("pool", e)

            @block.sync
            def _(e):
                run_stream("sp", e, extra_final=final_wait_ops)


def _g128(h):
    lg = math.log1p(-(2.0 ** (-5.0 - h)))
    return math.exp(128.0 * lg)


def build(L, NT=NT):
    nc = bass.Bass("TRN2", target_bir_lowering=False)
    x_in = nc.dram_tensor("x_in", [NT, NCH, 128, D], F32, kind="ExternalInput").ap()
    y_out = nc.dram_tensor("y_out", [NT, NCH, 128, D], F32, kind="ExternalOutput").ap()
    posi_d = nc.dram_tensor("posi", [128, NT * NCH], I32, kind="ExternalInput").ap()
    w_in_d = nc.dram_tensor("w_in", [L, 16, 128, KC * 512], F32, kind="ExternalInput").ap()
    w_out_d = nc.dram_tensor("w_out", [L, 128, 16 * D], F32, kind="ExternalInput").ap()
    gpre_d = nc.dram_tensor("gpre", [128, L * 8], F32, kind="ExternalInput").ap()
    gpost_d = nc.dram_tensor("gpost", [128, L * 8], F32, kind="ExternalInput").ap()
    cw_d = nc.dram_tensor("cw", [128, L * 24], F32, kind="ExternalInput").ap()
    cst_d = nc.dram_tensor("cst", [128, C_TOT], F32, kind="ExternalInput").ap()

    A = nc.alloc_sbuf_tensor
    xT = A("xT", [128, KC, T], F32)
    hT = A("hT", [128, KC, T], BF16)
    mixT = A("mixT", [128, 16, T], BF16)
    wout = A("wout", [128, 16, D], BF16)
    win = [A(f"win{i}", [128, KC, 512], BF16) for i in range(3)]
    Ast = A("Ast", [128, L * 8, 128], F32)
    Ubf = [A(f"Ubf{i}", [128, 128], BF16) for i in range(4)]
    utail = A("utail", [128, L * 8, 2], F32)
    o_sb = A("o_sb", [128, 8, 256], F32)
    sqn = A("sqn", [128, 8, 256], BF16)
    xs = o_sb[:, 0:4, :].rearrange("p c t -> p (c t)")
    XS = [("o_sb", i) for i in range(4)]
    rstd_n = A("rstd_n", [128, 256], F32)
    rstd_p = [A(f"rstd_p{i}", [128, 128], F32) for i in range(2)]
    cosF = A("cosF", [128, NCH, 128], F32)
    sinS = A("sinS", [128, NCH, 128], F32)
    ang = A("ang", [128, 64], F32)
    ki = A("ki", [128, 64], I32)
    kfl = A("kfl", [128, 64], F32)
    posi = A("posi_sb", [128, NT * NCH], I32)
    posf = A("posf", [128, NT * NCH], F32)
    cst = A("cst_sb", [128, C_TOT], F32)
    gpre = A("gpre_sb", [128, L * 8], F32)
    gpost = A("gpost_sb", [128, L * 8], F32)
    cw = A("cw_sb", [128, L * 24], F32)
    ident_bf = A("ident_bf", [128, 128], BF16)
    o128 = A("o128", [128, 128], BF16)
    o1024 = A("o1024", [128, 128], BF16)
    cx_sb = A("cx_sb", [128, 512], F32)
    ubuf = [A(f"ubuf{i}", [128, 514], F32) for i in range(2)]
    gate = [A(f"gate{i}", [128, 512], F32) for i in range(2)]
    ycv = A("ycv", [128, 512], F32)
    zs = gate
    t1 = [A(f"t1_{i}", [128, 128], F32) for i in range(2)]
    t2 = [A(f"t2_{i}", [128, 128], F32) for i in range(2)]
    t3 = [A(f"t3_{i}", [128, 128], F32) for i in range(2)]
    t4 = [A(f"t4_{i}", [128, 128], F32) for i in range(2)]
    qr = [A(f"qr{i}", [128, 128], BF16) for i in range(2)]
    kr = [A(f"kr{i}", [128, 128], BF16) for i in range(2)]
    v_sb = [A(f"v_sb{i}", [128, 128], BF16) for i in range(6)]
    RT_sb = [A(f"RT_sb{i}", [128, 128], F32) for i in range(3)]
    qT_sb = [A(f"qT_sb{i}", [128, 128], BF16) for i in range(3)]
    kT_sb = [A(f"kT_sb{i}", [128, 128], BF16) for i in range(3)]
    PT = [A(f"PT{i}", [128, 128], BF16) for i in range(2)]
    sqr = [A(f"sqr{i}", [128, 128], BF16) for i in range(2)]
    rl = [A(f"rl{i}", [128, 128], F32) for i in range(2)]
    rn = [A(f"rn{i}", [128, 128], F32) for i in range(2)]

    P = nc.alloc_psum_tensor
    bank = [P(f"bank{i}", [128, 512], F32) for i in range(8)]
    bank4_bf = bank[4][:, :].bitcast(BF16)

    def pk(b, q0=0, q1=4):
        return [("ps", b, q) for q in range(q0, q1)]

    S = Sched(nc)

    S.op("sp", lambda e: e.dma_start(out=cst[:], in_=cst_d), writes=["cst"], dma="c0")
    S.op("sp", lambda e: e.dma_start(out=gpre[:], in_=gpre_d), writes=["gpre"], dma="c1")
    S.op("sp", lambda e: e.dma_start(out=gpost[:], in_=gpost_d), writes=["gpost"], dma="c2")
    S.op("sp", lambda e: e.dma_start(out=cw[:], in_=cw_d), writes=["cw"], dma="c3")
    S.op("sp", lambda e: e.dma_start(out=posi[:], in_=posi_d), writes=["posi"], dma="c4")
    S.op("dve", lambda e: e.tensor_copy(out=ident_bf[:], in_=cst[:, C_ID:C_ID + 128]), reads=["cst"], writes=["ident_bf"])
    S.op("dve", lambda e: e.tensor_copy(out=o128[:], in_=cst[:, C_O128:C_O128 + 128]), reads=["cst"], writes=["o128"])
    S.op("dve", lambda e: e.tensor_copy(out=o1024[:], in_=cst[:, C_O1024:C_O1024 + 128]), reads=["cst"], writes=["o1024"])
    S.op("dve", lambda e: e.tensor_copy(out=posf[:], in_=posi[:]), reads=["posi"], writes=["posf"])
    S.op("pool", lambda e: e.memset(Ast[:], 0.0), writes=[("A", i) for i in range(L * 8)])
    S.op("pool", lambda e: e.memset(utail[:], 0.0), writes=[("utail", i) for i in range(L * 8)])
    identf = cst[:, C_ID:C_ID + 128]
    maskT = cst[:, C_MASK:C_MASK + 128]
    invf = cst[:, C_INVF:C_INVF + 64]

    out_dmas = []
    head_seq = [(ti_, l_, hd_) for ti_ in range(NT) for l_ in range(L) for hd_ in range(16)]
    n_loaded = [0]

    def ensure_loaded(upto):
        while n_loaded[0] <= min(upto, len(head_seq) - 1):
            n = n_loaded[0]
            _, l_, hd_ = head_seq[n]
            i = n % 3
            S.op("pool", lambda e, l_=l_, hd_=hd_, i=i: e.dma_start(
                out=win[i][:], in_=w_in_d[l_, hd_].rearrange("p (k c) -> p k c", k=KC)),
                writes=[("win", i)], dma=f"win{i}")
            n_loaded[0] += 1

    def start_head(ti_, l_, hd_):
        n = (ti_ * L + l_) * 16 + hd_
        ensure_loaded(n + 2)
        S.op("pool", lambda e, l_=l_, hd_=hd_: e.dma_start(out=wout[:, hd_, :], in_=w_out_d[l_][:, hd_ * D:(hd_ + 1) * D]),
             writes=[("wout", hd_)], dma=f"wout{hd_}")
        return n % 3

    ensure_loaded(1)

    for ti in range(NT):
        xt_ps = [bank[i][:, :].rearrange("p (k c) -> p k c", k=4) for i in range(4)]
        xs2 = [o_sb[:, 0:4, :].rearrange("p c t -> p (c t)"), o_sb[:, 4:8, :].rearrange("p c t -> p (c t)")]
        XS2 = [[("o_sb", i) for i in range(4)], [("o_sb", 4 + i) for i in range(4)]]
        for c in range(NCH):
            p = c % 2
            S.op("sp", lambda e, ti=ti, c=c, p=p: e.dma_start(out=xs2[p], in_=x_in[ti, c]), writes=XS2[p], dma=f"xs{p}")
            for kc in range(KC):
                S.op("pe", lambda e, kc=kc, p=p: e.transpose(out=xt_ps[2 * p + kc // 4][:, kc % 4, :], in_=xs2[p][:, kc * 128:(kc + 1) * 128],
                                                             identity=identf),
                     reads=XS2[p] + ["cst"], writes=pk(2 * p + kc // 4, kc % 4, kc % 4 + 1))
            S.op("dve", lambda e, c=c, p=p: e.tensor_copy(out=xT[:, 0:4, c * 128:(c + 1) * 128], in_=xt_ps[2 * p]),
                 reads=pk(2 * p), writes=[("xT", c // 2)])
            S.op("act", lambda e, c=c, p=p: e.copy(out=xT[:, 4:8, c * 128:(c + 1) * 128], in_=xt_ps[2 * p + 1]),
                 reads=pk(2 * p + 1), writes=[("xT", c // 2)])
        for c in range(NCH):
            gc = ti * NCH + c
            S.op("dve", lambda e, gc=gc: e.tensor_scalar(out=ang[:], in0=invf, scalar1=posf[:, gc:gc + 1], scalar2=None, op0=ALU.mult),
                 reads=["cst", "posf"], writes=["ang"])
            S.op("dve", lambda e: e.tensor_scalar(out=ki[:], in0=ang[:], scalar1=1.0 / TWO_PI, scalar2=None, op0=ALU.mult),
                 reads=["ang"], writes=["ki"])
            S.op("dve", lambda e: e.tensor_copy(out=kfl[:], in_=ki[:]), reads=["ki"], writes=["kfl"])
            S.op("dve", lambda e: e.scalar_tensor_tensor(out=ang[:], in0=kfl[:], scalar=-CW1, in1=ang[:], op0=ALU.mult, op1=ALU.add),
                 reads=["kfl", "ang"], writes=["ang"])
            S.op("dve", lambda e: e.scalar_tensor_tensor(out=ang[:], in0=kfl[:], scalar=-CW2, in1=ang[:], op0=ALU.mult, op1=ALU.add),
                 reads=["kfl", "ang"], writes=["ang"])
            S.op("dve", lambda e, c=c: e.tensor_scalar(out=sinS[:, c, 64:128], in0=ang[:], scalar1=-PI_SAFE, scalar2=PI_SAFE,
                                                      op0=ALU.max, op1=ALU.min),
                 reads=["ang"], writes=["sinS"])
        S.op("act", lambda e: e.activation(out=cosF[:, :, 64:128], in_=sinS[:, :, 64:128], func=AF.Abs),
             reads=["sinS"], writes=["cosF"])
        S.op("act", lambda e: e.activation(out=sinS[:, :, 0:64], in_=sinS[:, :, 64:128], func=AF.Sin, scale=-1.0),
             reads=["sinS"], writes=["sinS"])
        S.op("act", lambda e: e.activation(out=sinS[:, :, 64:128], in_=sinS[:, :, 64:128], func=AF.Sin),
             reads=["sinS"], writes=["sinS"])
        S.op("act", lambda e: e.activation(out=cosF[:, :, 0:64], in_=cosF[:, :, 64:128], func=AF.Sin, scale=-1.0, bias=cst[:, C_HALFPI:C_HALFPI + 1]),
             reads=["cosF", "cst"], writes=["cosF"])
        S.op("act", lambda e: e.copy(out=cosF[:, :, 64:128], in_=cosF[:, :, 0:64]), reads=["cosF"], writes=["cosF"])

        for l in range(L):
            def prenorm_granule(l_, gi):
                q2 = gi % 2
                t0_ = gi * 128
                sbk = gi // 2
                stg = (cx_sb if q2 == 0 else ycv)[:, :].bitcast(BF16).rearrange("p (k t) -> p k t", k=8)
                skey = "cx_sb" if q2 == 0 else "ycv"
                ps = bank[2 + q2][:, 256:384]
                pkeys = pk(2 + q2, 2, 3)
                S.op("act", lambda e: e.activation(out=stg, in_=xT[:, :, t0_:t0_ + 128], func=AF.Square),
                     reads=[("xT", sbk)], writes=[skey])
                for kc in range(KC):
                    S.op("pe", lambda e, kc=kc: e.matmul(ps, lhsT=o1024[:], rhs=stg[:, kc, :], start=(kc == 0), stop=(kc == KC - 1),
                                                         skip_group_check=True),
                         reads=[skey, "o1024"], writes=pkeys)
                S.op("act", lambda e: e.activation(out=rstd_p[q2][:], in_=ps, func=AF.Ln, bias=cst[:, C_EPS:C_EPS + 1]),
                     reads=pkeys + ["cst"], writes=[("rstd_p", q2)])
                S.op("act", lambda e: e.activation(out=rstd_p[q2][:], in_=rstd_p[q2][:], func=AF.Exp, scale=-0.5),
                     reads=[("rstd_p", q2)], writes=[("rstd_p", q2)])
                for kc in range(KC):
                    S.op("dve", lambda e, kc=kc: e.scalar_tensor_tensor(
                        out=hT[:, kc, t0_:t0_ + 128], in0=xT[:, kc, t0_:t0_ + 128], scalar=gpre[:, l_ * 8 + kc:l_ * 8 + kc + 1],
                        in1=rstd_p[q2][:], op0=ALU.mult, op1=ALU.mult),
                         reads=[("xT", sbk), "gpre", ("rstd_p", q2)], writes=[("hT", sbk)])

            for sb in range(4):
                t0 = sb * 256
                nh = sb % 2
                S.op("act", lambda e, t0=t0: e.activation(out=sqn[:], in_=xT[:, :, t0:t0 + 256], func=AF.Square),
                     reads=[("xT", sb)], writes=["sqn"])
                for kc in range(KC):
                    S.op("pe", lambda e, kc=kc, nh=nh: e.matmul(bank[nh][:, 0:256], lhsT=o1024[:], rhs=sqn[:, kc, :],
                                                               start=(kc == 0), stop=(kc == KC - 1)),
                         reads=["sqn", "o1024"], writes=pk(nh, 0, 2))
                S.op("act", lambda e, nh=nh: e.activation(out=rstd_n[:], in_=bank[nh][:, 0:256], func=AF.Ln,
                                                         bias=cst[:, C_EPS:C_EPS + 1]),
                     reads=pk(nh, 0, 2) + ["cst"], writes=["rstd_n"])
                S.op("act", lambda e: e.activation(out=rstd_n[:], in_=rstd_n[:], func=AF.Exp, scale=-0.5),
                     reads=["rstd_n"], writes=["rstd_n"])
                for kc in range(KC):
                    S.op("dve", lambda e, kc=kc, t0=t0, l=l: e.scalar_tensor_tensor(
                        out=hT[:, kc, t0:t0 + 256], in0=xT[:, kc, t0:t0 + 256], scalar=gpre[:, l * 8 + kc:l * 8 + kc + 1],
                        in1=rstd_n[:], op0=ALU.mult, op1=ALU.mult),
                         reads=[("xT", sb), "gpre", "rstd_n"], writes=[("hT", sb)])

            conv_units = [(g, b) for g in range(8) for b in range(2)]
            wbuf_of = {}

            def conv_c0(u):
                g, b = conv_units[u]
                if b == 0:
                    wbuf_of[g] = start_head(ti, l, g)
                wi = wbuf_of[g]
                B = [0, 1, 2, 3] if u % 2 == 0 else [4, 5, 6, 7]
                for s in range(4):
                    for kc in range(KC):
                        S.op("pe", lambda e, s=s, kc=kc, wi=wi, b=b, B=B: e.matmul(
                            bank[B[s]][:, :], lhsT=win[wi][:, kc, s * 128:(s + 1) * 128], rhs=hT[:, kc, b * 512:(b + 1) * 512],
                            start=(kc == 0), stop=(kc == KC - 1)),
                             reads=[("win", wi), ("hT", 2 * b), ("hT", 2 * b + 1)], writes=pk(B[s]))

            def conv_c1(u):
                g, b = conv_units[u]
                B = [0, 1, 2, 3] if u % 2 == 0 else [4, 5, 6, 7]
                p = u % 2
                li = l * 8 + g
                S.op("act", lambda e, B=B: e.copy(out=cx_sb[:], in_=bank[B[2]][:, :]), reads=pk(B[2]), writes=["cx_sb"])
                S.op("act", lambda e, B=B, p=p: e.activation(out=gate[p][:], in_=bank[B[3]][:, :], func=AF.Exp, scale=-1.0),
                     reads=pk(B[3]), writes=[("gate", p)])
                S.op("act", lambda e, p=p: e.activation(out=gate[p][:], in_=gate[p][:], func=AF.Ln, bias=cst[:, C_ONE:C_ONE + 1]),
                     reads=[("gate", p), "cst"], writes=[("gate", p)])
                S.op("act", lambda e, p=p: e.activation(out=gate[p][:], in_=gate[p][:], func=AF.Exp, scale=-1.0),
                     reads=[("gate", p)], writes=[("gate", p)])
                if b == 0:
                    S.op("pool", lambda e, p=p, li=li: e.tensor_copy(out=ubuf[p][:, 0:2], in_=utail[:, li, :]),
                         reads=[("utail", li)], writes=[("ubuf", p)])
                else:
                    S.op("pool", lambda e, p=p: e.tensor_copy(out=ubuf[p][:, 0:2], in_=ubuf[1 - p][:, 512:514]),
                         reads=[("ubuf", 1 - p)], writes=[("ubuf", p)])
                S.op("dve", lambda e, B=B, p=p: e.tensor_tensor(out=ubuf[p][:, 2:514], in0=bank[B[1]][:, :], in1=cx_sb[:], op=ALU.mult),
                     reads=pk(B[1]) + ["cx_sb"], writes=[("ubuf", p)])
                if b == 1:
                    S.op("pool", lambda e, p=p, li=li: e.tensor_copy(out=utail[:, li, :], in_=ubuf[p][:, 512:514]),
                         reads=[("ubuf", p)], writes=[("utail", li)])
                S.op("dve", lambda e, B=B, p=p: e.tensor_tensor(out=gate[p][:], in0=bank[B[3]][:, :], in1=gate[p][:], op=ALU.mult),
                     reads=pk(B[3]) + [("gate", p)], writes=[("gate", p)])
                S.op("dve", lambda e, B=B, p=p: e.tensor_tensor(out=gate[p][:], in0=bank[B[0]][:, :], in1=gate[p][:], op=ALU.mult),
                     reads=pk(B[0]) + [("gate", p)], writes=[("gate", p)])

            def conv_c2(u):
                g, b = conv_units[u]
                p = u % 2
                li = l * 8 + g
                base = li * 3
                S.op("act", lambda e, p=p, base=base: e.activation(out=ycv[:], in_=ubuf[p][:, 2:514], func=AF.Copy,
                                                                  scale=cw[:, base + 2:base + 3]),
                     reads=[("ubuf", p), "cw"], writes=["ycv"])
                S.op("dve", lambda e, p=p, base=base: e.scalar_tensor_tensor(out=ycv[:], in0=ubuf[p][:, 1:513], scalar=cw[:, base + 1:base + 2],
                                                                             in1=ycv[:], op0=ALU.mult, op1=ALU.add),
                     reads=[("ubuf", p), "cw", "ycv"], writes=["ycv"])
                S.op("dve", lambda e, p=p, base=base: e.scalar_tensor_tensor(out=ycv[:], in0=ubuf[p][:, 0:512], scalar=cw[:, base:base + 1],
                                                                             in1=ycv[:], op0=ALU.mult, op1=ALU.add),
                     reads=[("ubuf", p), "cw", "ycv"], writes=["ycv"])
                S.op("pool", lambda e, p=p, g=g, b=b: e.tensor_tensor(out=mixT[:, g, b * 512:(b + 1) * 512], in0=ycv[:], in1=gate[p][:], op=ALU.mult),
                     reads=["ycv", ("gate", p)], writes=[("mixT", g, 2 * b), ("mixT", g, 2 * b + 1)])

            stages = [conv_c0, conv_c1, conv_c2]
            nU = len(conv_units)
            for step in range(nU + len(stages) - 1):
                for k in range(len(stages) - 1, -1, -1):
                    u = step - k
                    if 0 <= u < nU:
                        stages[k](u)

            ret_units = [(h, c) for h in range(8) for c in range(NCH)]

            def u_init(slot, h):
                li_ = l * 8 + h
                S.op("dve", lambda e: e.tensor_scalar(out=Ubf[slot][:], in0=Ast[:, li_, :], scalar1=_g128(h), scalar2=None, op0=ALU.mult),
                     reads=[("A", li_)], writes=[("U", slot)])

            def ret_b0(u):
                h, c = ret_units[u]
                if c == 0:
                    wbuf_of[8 + h] = start_head(ti, l, 8 + h)
                wi = wbuf_of[8 + h]
                tb = u % 2
                for kc in range(KC):
                    S.op("pe", lambda e, kc=kc: e.matmul(
                        bank[tb][:, 0:384], lhsT=hT[:, kc, c * 128:(c + 1) * 128], rhs=win[wi][:, kc, 0:384],
                        start=(kc == 0), stop=(kc == KC - 1)),
                         reads=[("win", wi), ("hT", c // 2)], writes=pk(tb, 0, 3))

            def ret_b1(u):
                h, c = ret_units[u]
                tb = u % 2
                r2 = u % 2
                r6 = u % 6
                TM = bank[tb]
                rd = pk(tb, 0, 3)
                qcol = cst[:, C_QW + h:C_QW + h + 1]
                kcol = cst[:, C_KW + h:C_KW + h + 1]
                S.op("act", lambda e: e.copy(out=v_sb[r6][:], in_=TM[:, 256:384]), reads=rd, writes=[("v_sb", r6)])
                S.op("dve", lambda e: e.scalar_tensor_tensor(out=t1[r2][:], in0=TM[:, 0:128], scalar=qcol, in1=cosF[:, c, :],
                                                             op0=ALU.mult, op1=ALU.mult),
                     reads=rd + ["cst", "cosF"], writes=[("t1", r2)])
                S.op("dve", lambda e: e.scalar_tensor_tensor(
                    out=t2[r2][:, :].rearrange("p (a b) -> p a b", a=2), in0=TM[:, 0:128].rearrange("p (a b) -> p a b", a=2)[:, ::-1, :],
                    scalar=qcol, in1=sinS[:, c, :].rearrange("p (a b) -> p a b", a=2), op0=ALU.mult, op1=ALU.mult),
                     reads=rd + ["cst", "sinS"], writes=[("t2", r2)])
                S.op("dve", lambda e: e.scalar_tensor_tensor(out=t3[r2][:], in0=TM[:, 128:256], scalar=kcol, in1=cosF[:, c, :],
                                                             op0=ALU.mult, op1=ALU.mult),
                     reads=rd + ["cst", "cosF"], writes=[("t3", r2)])
                S.op("dve", lambda e: e.scalar_tensor_tensor(
                    out=t4[r2][:, :].rearrange("p (a b) -> p a b", a=2), in0=TM[:, 128:256].rearrange("p (a b) -> p a b", a=2)[:, ::-1, :],
                    scalar=kcol, in1=sinS[:, c, :].rearrange("p (a b) -> p a b", a=2), op0=ALU.mult, op1=ALU.mult),
                     reads=rd + ["cst", "sinS"], writes=[("t4", r2)])

            def ret_b2(u):
                h, c = ret_units[u]
                r2 = u % 2
                S.op("pool", lambda e: e.tensor_tensor(out=qr[r2][:], in0=t1[r2][:], in1=t2[r2][:], op=ALU.add),
                     reads=[("t1", r2), ("t2", r2)], writes=[("qr", r2)])
                S.op("pool", lambda e: e.tensor_tensor(out=kr[r2][:], in0=t3[r2][:], in1=t4[r2][:], op=ALU.add),
                     reads=[("t3", r2), ("t4", r2)], writes=[("kr", r2)])

            def ret_b3(u):
                h, c = ret_units[u]
                r2 = u % 2
                r3 = u % 3
                r6 = u % 6
                li = l * 8 + h
                g = _g128(h)
                qT_ps = bank4_bf[:, 0:128]
                kT_ps = bank4_bf[:, 128:256]
                KV = bank[5][:, 128:256]
                S.op("pe", lambda e: e.transpose(out=qT_ps, in_=qr[r2][:], identity=ident_bf[:]),
                     reads=[("qr", r2), "ident_bf"], writes=pk(4, 0, 1))
                S.op("pe", lambda e: e.transpose(out=kT_ps, in_=kr[r2][:], identity=ident_bf[:]),
                     reads=[("kr", r2), "ident_bf"], writes=pk(4, 1, 2))
                S.op("pe", lambda e: e.matmul(KV, lhsT=kr[r2][:], rhs=v_sb[r6][:], start=True, stop=True, skip_group_check=True),
                     reads=[("kr", r2), ("v_sb", r6)], writes=pk(5, 1, 2))
                S.op("act", lambda e: e.copy(out=qT_sb[r3][:], in_=qT_ps), reads=pk(4, 0, 1), writes=[("qT_sb", r3)])
                S.op("act", lambda e: e.copy(out=kT_sb[r2][:], in_=kT_ps), reads=pk(4, 1, 2), writes=[("kT_sb", r2)])
                S.op("dve", lambda e: e.scalar_tensor_tensor(out=Ast[:, li, :], in0=Ast[:, li, :], scalar=g, in1=KV, op0=ALU.mult, op1=ALU.add),
                     reads=[("A", li)] + pk(5, 1, 2), writes=[("A", li)])
                if c < NCH - 1:
                    S.op("dve", lambda e: e.tensor_scalar(out=Ubf[(u + 1) % 4][:], in0=Ast[:, li, :], scalar1=g, scalar2=None, op0=ALU.mult),
                         reads=[("A", li)], writes=[("U", (u + 1) % 4)])
                elif h < 7:
                    u_init((u + 1) % 4, h + 1)
                zb = (u // 4) % 2
                if c % 4 == 0:
                    b = c // 4
                    wi = wbuf_of[8 + h]
                    for kc in range(KC):
                        S.op("pe", lambda e, kc=kc: e.matmul(bank[3][:, :], lhsT=win[wi][:, kc, 384:512], rhs=hT[:, kc, b * 512:(b + 1) * 512],
                                                             start=(kc == 0), stop=(kc == KC - 1)),
                             reads=[("win", wi), ("hT", 2 * b), ("hT", 2 * b + 1)], writes=pk(3))
                    S.op("act", lambda e: e.activation(out=zs[zb][:], in_=bank[3][:, :], func=AF.Exp, scale=-1.0),
                         reads=pk(3), writes=[("gate", zb)])
                elif c % 4 == 1:
                    S.op("act", lambda e: e.activation(out=zs[zb][:], in_=zs[zb][:], func=AF.Ln, bias=cst[:, C_ONE:C_ONE + 1]),
                         reads=[("gate", zb), "cst"], writes=[("gate", zb)])
                elif c % 4 == 2:
                    S.op("act", lambda e: e.activation(out=zs[zb][:], in_=zs[zb][:], func=AF.Exp, scale=-1.0),
                         reads=[("gate", zb)], writes=[("gate", zb)])
                else:
                    S.op("dve", lambda e: e.tensor_tensor(out=zs[zb][:], in0=bank[3][:, :], in1=zs[zb][:], op=ALU.mult),
                         reads=pk(3) + [("gate", zb)], writes=[("gate", zb)])

            def ret_b4(u):
                h, c = ret_units[u]
                r2 = u % 2
                r3 = u % 3
                ST = bank[5][:, 0:128]
                S.op("pe", lambda e: e.matmul(ST, lhsT=kT_sb[r2][:], rhs=qT_sb[r3][:], start=True, stop=True, skip_group_check=True),
                     reads=[("kT_sb", r2), ("qT_sb", r3)], writes=pk(5, 0, 1))
                S.op("dve", lambda e: e.tensor_tensor(out=PT[r2][:], in0=ST, in1=maskT, op=ALU.mult),
                     reads=pk(5, 0, 1) + ["cst"], writes=[("PT", r2)])

            def ret_b5(u):
                h, c = ret_units[u]
                r2 = u % 2
                r3 = u % 3
                r6 = u % 6
                RT = bank[6 + r2][:, 0:128]
                kRT = pk(6 + r2, 0, 1)
                S.op("pe", lambda e: e.matmul(RT, lhsT=v_sb[r6][:], rhs=PT[r2][:], start=True, stop=False),
                     reads=[("v_sb", r6), ("PT", r2)], writes=kRT)
                S.op("pe", lambda e: e.matmul(RT, lhsT=Ubf[u % 4][:], rhs=qT_sb[r3][:], start=False, stop=True),
                     reads=[("U", u % 4), ("qT_sb", r3)], writes=kRT)

            def ret_b6(u):
                r2 = u % 2
                r3 = u % 3
                RT = bank[6 + r2][:, 0:128]
                kRT = pk(6 + r2, 0, 1)
                S.op("act", lambda e: e.activation(out=sqr[r2][:], in_=RT, func=AF.Square), reads=kRT, writes=[("sqr", r2)])
                S.op("act", lambda e: e.copy(out=RT_sb[r3][:], in_=RT), reads=kRT, writes=[("RT_sb", r3)])

            def ret_b7(u):
                r2 = u % 2
                RS = bank[2][:, 0:128]
                S.op("pe", lambda e: e.matmul(RS, lhsT=o128[:], rhs=sqr[r2][:], start=True, stop=True),
                     reads=["o128", ("sqr", r2)], writes=pk(2, 0, 1))
                S.op("act", lambda e: e.activation(out=rl[r2][:], in_=RS, func=AF.Ln, bias=cst[:, C_EPS:C_EPS + 1]),
                     reads=pk(2, 0, 1) + ["cst"], writes=[("rl", r2)])
                S.op("act", lambda e: e.activation(out=rl[r2][:], in_=rl[r2][:], func=AF.Exp, scale=-0.5),
                     reads=[("rl", r2)], writes=[("rl", r2)])

            def ret_b8(u):
                h, c = ret_units[u]
                r2 = u % 2
                r3 = u % 3
                zb = (u // 4) % 2
                S.op("pool", lambda e: e.tensor_tensor(out=rn[r2][:], in0=RT_sb[r3][:], in1=rl[r2][:], op=ALU.mult),
                     reads=[("RT_sb", r3), ("rl", r2)], writes=[("rn", r2)])
                S.op("pool", lambda e: e.tensor_tensor(out=mixT[:, 8 + h, c * 128:(c + 1) * 128], in0=rn[r2][:],
                                                       in1=zs[zb][:, (c % 4) * 128:(c % 4 + 1) * 128], op=ALU.mult),
                     reads=[("rn", r2), ("gate", zb)], writes=[("mixT", 8 + h, c // 2)])

            u_init(0, 0)
            stages = [ret_b0, ret_b1, ret_b2, ret_b3, ret_b4, ret_b5, ret_b6, ret_b7, ret_b8]
            order = [2, 8, 3, 7, 6, 5, 4, 1, 0]
            nU = len(ret_units)
            for step in range(nU + len(stages) - 1):
                for k in order:
                    u = step - k
                    if 0 <= u < nU:
                        stages[k](u)

            for sb in range(4):
                t0 = sb * 256
                nh = sb % 2
                for cc in range(8):
                    ob = bank[cc][:, 0:256]
                    okeys = pk(cc, 0, 2)
                    for fc in range(16):
                        S.op("pe", lambda e, ob=ob, fc=fc, cc=cc, t0=t0: e.matmul(
                            ob, lhsT=wout[:, fc, cc * 128:(cc + 1) * 128], rhs=mixT[:, fc, t0:t0 + 256],
                            start=(fc == 0), stop=(fc == 15)),
                             reads=[("wout", fc), ("mixT", fc, sb)], writes=okeys)
                    S.op("act", lambda e, ob=ob, cc=cc: e.copy(out=o_sb[:, cc, :], in_=ob), reads=okeys, writes=[("o_sb", cc)])
                    S.op("act", lambda e, ob=ob, cc=cc: e.activation(out=sqn[:, cc, :], in_=ob, func=AF.Square), reads=okeys, writes=["sqn"])
                for cc in range(8):
                    S.op("pe", lambda e, cc=cc, nh=nh: e.matmul(bank[nh][:, 256:512], lhsT=o1024[:], rhs=sqn[:, cc, :],
                                                               start=(cc == 0), stop=(cc == 7)),
                         reads=["sqn", "o1024"], writes=pk(nh, 2, 4))
                S.op("act", lambda e, nh=nh: e.activation(out=rstd_n[:], in_=bank[nh][:, 256:512], func=AF.Ln,
                                                         bias=cst[:, C_EPS:C_EPS + 1]),
                     reads=pk(nh, 2, 4) + ["cst"], writes=["rstd_n"])
                S.op("act", lambda e: e.activation(out=rstd_n[:], in_=rstd_n[:], func=AF.Exp, scale=-0.5),
                     reads=["rstd_n"], writes=["rstd_n"])
                for cc in range(8):
                    S.op("dve", lambda e, cc=cc, l=l: e.scalar_tensor_tensor(
                        out=o_sb[:, cc, :], in0=o_sb[:, cc, :], scalar=gpost[:, l * 8 + cc:l * 8 + cc + 1], in1=rstd_n[:],
                        op0=ALU.mult, op1=ALU.mult),
                         reads=[("o_sb", cc), "gpost", "rstd_n"], writes=[("o_sb", cc)])
                    S.op("pool", lambda e, cc=cc, t0=t0: e.tensor_tensor(out=xT[:, cc, t0:t0 + 256], in0=xT[:, cc, t0:t0 + 256],
                                                                        in1=o_sb[:, cc, :], op=ALU.add),
                         reads=[("o_sb", cc), ("xT", sb)], writes=[("xT", sb)])

        for c in range(NCH):
            p = c % 2
            for kc in range(KC):
                S.op("pe", lambda e, kc=kc, c=c, p=p: e.transpose(out=xt_ps[2 * p + kc // 4][:, kc % 4, :], in_=xT[:, kc, c * 128:(c + 1) * 128],
                                                                  identity=identf),
                     reads=[("xT", c // 2), "cst"], writes=pk(2 * p + kc // 4, kc % 4, kc % 4 + 1))
            S.op("dve", lambda e, p=p: e.tensor_copy(out=xs2[p][:, 0:512], in_=bank[2 * p][:, :]), reads=pk(2 * p), writes=XS2[p])
            S.op("act", lambda e, p=p: e.copy(out=xs2[p][:, 512:1024], in_=bank[2 * p + 1][:, :]), reads=pk(2 * p + 1), writes=XS2[p])
            out_dmas.append(S.op("sp", lambda e, ti=ti, c=c, p=p: e.dma_start(out=y_out[ti, c], in_=xs2[p]), reads=XS2[p], dma=f"ys{p}"))

    S.emit(final_wait_ops=[out_dmas[-2], out_dmas[-1]])
    return nc


def _consts():
    cst = np.zeros((128, C_TOT), np.float32)
    cst[:, C_ID:C_ID + 128] = np.eye(128, dtype=np.float32)
    j = np.arange(128)[:, None]
    i = np.arange(128)[None, :]
    cst[:, C_MASK:C_MASK + 128] = (i >= j).astype(np.float32)
    cst[:, C_O128:C_O128 + 128] = 1.0 / 128.0
    cst[:, C_O1024:C_O1024 + 128] = 1.0 / 1024.0
    lg = np.log1p(-np.exp2(-5.0 - np.arange(8, dtype=np.float64)))
    idx = np.arange(128, dtype=np.float64)[:, None]
    cst[:, C_QW:C_QW + 8] = np.exp((idx + 1.0) * lg[None, :]).astype(np.float32)
    cst[:, C_KW:C_KW + 8] = (np.exp(-(idx + 1.0) * lg[None, :]) * (128.0 ** -0.5)).astype(np.float32)
    half = 64
    inv_freq = (1.0 / (np.float32(10000.0) ** (np.arange(half, dtype=np.float32) / np.float32(half)))).astype(np.float32)
    cst[:, C_INVF:C_INVF + 64] = inv_freq[None, :]
    cst[:, C_HALFPI] = math.pi / 2
    cst[:, C_EPS] = EPS
    cst[:, C_ONE] = 1.0
    return cst


_NC_CACHE = {}


def _get_nc(L):
    if L not in _NC_CACHE:
        _NC_CACHE[L] = build(L)
    return _NC_CACHE[L]


def _layout_weights(w_in, w_out, pre_norm, post_norm, conv_w):
    L = w_in.shape[0]
    wi = np.ascontiguousarray(
        w_in.reshape(L, 8, 128, 2, 4, 8, 128).transpose(0, 3, 5, 2, 1, 4, 6)).reshape(L, 16, 128, KC * 512)
    wo = np.ascontiguousarray(w_out.reshape(L, 16, 128, D).transpose(0, 2, 1, 3)).reshape(L, 128, 16 * D)
    gpre = np.ascontiguousarray(pre_norm.reshape(L, 8, 128).transpose(2, 0, 1)).reshape(128, L * 8)
    gpost = np.ascontiguousarray(post_norm.reshape(L, 8, 128).transpose(2, 0, 1)).reshape(128, L * 8)
    cw = np.ascontiguousarray(conv_w.reshape(L, 3, 8, 128).transpose(3, 0, 2, 1)).reshape(128, L * 24)
    return wi, wo, gpre, gpost, cw


FUSED = True


def kernel(x, positions, pre_norm, w_in, conv_w, w_out, post_norm):
    x = np.asarray(x, np.float32)
    positions = np.asarray(positions, np.int32)
    B = x.shape[0]
    cst = _consts()
    posl = [np.ascontiguousarray(positions[b].reshape(NT * NCH, 128).T) for b in range(B)]
    groups = [list(range(DEPTH))] if FUSED else [[l] for l in range(DEPTH)]
    cur = x
    for layers in groups:
        L = len(layers)
        nc = _get_nc(L)
        wi, wo, gpre, gpost, cw = _layout_weights(
            np.asarray(w_in, np.float32)[layers], np.asarray(w_out, np.float32)[layers],
            np.asarray(pre_norm, np.float32)[layers], np.asarray(post_norm, np.float32)[layers],
            np.asarray(conv_w, np.float32)[layers])
        in_maps = []
        for b in range(B):
            in_maps.append({"x_in": np.ascontiguousarray(cur[b].reshape(NT, NCH, 128, D)), "posi": posl[b], "w_in": wi,
                            "w_out": wo, "gpre": gpre, "gpost": gpost, "cw": cw, "cst": cst})
        res = run_bass_kernel_spmd(nc, in_maps, core_ids=list(range(B)))
        cur = np.stack([np.asarray(r["y_out"]).reshape(SEQ, D) for r in res.results], axis=0)
    return cur.astype(np.float32)
```
